# Optimizing a Trainium2 kernel written in Bass

```python
import jax, jax.numpy as jnp
from jax import lax
import numpy as np

D_MODEL = 2048
BATCH = 1
SEQ = 8192
DEPTH = 1

HEAD_DIM = 128
N_SPARSE_HEADS = 8
SPARSE_WIDTH = N_SPARSE_HEADS * HEAD_DIM
N_IDX_HEADS = 16
IDX_HEAD_DIM = 64
TOPK_MAX = 256
DIL_GROUPS = ((128, 1), (512, 4), (2048, 16))
HEADS_PER_DIL_GROUP = 4
N_DIL_HEADS = HEADS_PER_DIL_GROUP * len(DIL_GROUPS)
DIL_WIDTH = N_DIL_HEADS * HEAD_DIM
DIL_OUT_WIDTH = HEADS_PER_DIL_GROUP * HEAD_DIM
N_ALIBI_HEADS = N_DIL_HEADS + N_SPARSE_HEADS
BLOCK = 128
D_FF = ((-(-8 * D_MODEL // 3)) + 255) // 256 * 256
DEEPNORM_ALPHA = (2 * DEPTH) ** 0.25
DEEPNORM_BETA = (8 * DEPTH) ** -0.25
LN_EPS = 1e-5
IN_SIZES = (SPARSE_WIDTH, SPARSE_WIDTH, SPARSE_WIDTH,
            N_IDX_HEADS * IDX_HEAD_DIM, IDX_HEAD_DIM, N_IDX_HEADS,
            DIL_WIDTH, DIL_WIDTH, DIL_WIDTH,
            D_MODEL, D_MODEL)
IN_WIDTH = sum(IN_SIZES)
V_COLUMN_SLOTS = (2, 8)

kernel_name = "hybrid_dsa_dilated_gated_deepnorm"


def layer_norm(x, g, b):
    xf = x.astype(jnp.float32)
    mu = jnp.mean(xf, -1, keepdims=True)
    var = jnp.mean(jnp.square(xf - mu), -1, keepdims=True)
    y = (xf - mu) * lax.rsqrt(var + LN_EPS) * g.astype(jnp.float32) + b.astype(jnp.float32)
    return y.astype(x.dtype)


def alibi_slopes():
    return jnp.exp2(-8.0 * jnp.arange(1, N_ALIBI_HEADS + 1, dtype=jnp.float32) / N_ALIBI_HEADS)


def dsa_attention(q, k, v, q_idx, k_idx, w_idx, slopes):
    B, S, H, Dh = q.shape
    topk = min(TOPK_MAX, S // 4)
    nb = S // BLOCK
    key_pos = jnp.arange(S)
    k_idx32 = k_idx.astype(jnp.float32)
    gather = jax.vmap(lambda a, i: a[i])

    def to_blocks(a):
        return jnp.moveaxis(a.reshape((B, nb, BLOCK) + a.shape[2:]), 1, 0)

    def block_fn(args):
        n, qb, qib, wb = args
        qpos = n * BLOCK + jnp.arange(BLOCK)
        rel = jnp.einsum('bqhd,bsd->bqhs', qib.astype(jnp.float32), k_idx32) * IDX_HEAD_DIM ** -0.5
        score = jnp.einsum('bqh,bqhs->bqs', wb.astype(jnp.float32), jax.nn.relu(rel))
        causal = key_pos[None, :] <= qpos[:, None]
        score = jnp.where(causal[None], score, -jnp.inf)
        _, sel = lax.top_k(score, topk)
        ks = gather(k, sel)
        vs = gather(v, sel)
        logits = jnp.einsum('bqhd,bqkhd->bhqk', qb, ks,
                            preferred_element_type=jnp.float32) * Dh ** -0.5
        dist = (qpos[None, :, None] - sel).astype(jnp.float32)
        logits = logits - slopes[None, :, None, None] * dist[:, None]
        valid = sel <= qpos[None, :, None]
        logits = jnp.where(valid[:, None], logits, -jnp.inf)
        p = jax.nn.softmax(logits, axis=-1).astype(v.dtype)
        return jnp.einsum('bhqk,bqkhd->bqhd', p, vs)

    out = lax.map(block_fn, (jnp.arange(nb), to_blocks(q), to_blocks(q_idx), to_blocks(w_idx)))
    return jnp.moveaxis(out, 0, 1).reshape(B, S, H * Dh)


def dilated_group_attention(q, k, v, window, dilation, slopes):
    B, S, h, Dh = q.shape
    span = window // dilation
    assert span <= BLOCK
    n_sub = -(-S // dilation)
    n_pad = -(-n_sub // BLOCK) * BLOCK
    s_pad = n_pad * dilation
    nb = n_pad // BLOCK

    def to_sub(a):
        a = jnp.pad(a, ((0, 0), (0, s_pad - S), (0, 0), (0, 0)))
        a = a.reshape(B, n_pad, dilation, h, Dh).transpose(0, 2, 1, 3, 4)
        return a.reshape(B * dilation, nb, BLOCK, h, Dh)

    def prev(a):
        return jnp.pad(a, ((0, 0), (1, 0), (0, 0), (0, 0), (0, 0)))[:, :-1]

    qs, ks, vs = to_sub(q), to_sub(k), to_sub(v)
    kk = jnp.concatenate([prev(ks), ks], axis=2)
    vv = jnp.concatenate([prev(vs), vs], axis=2)
    logits = jnp.einsum('znqhd,znkhd->zhnqk', qs, kk,
                        preferred_element_type=jnp.float32) * Dh ** -0.5
    step = BLOCK + jnp.arange(BLOCK)[:, None] - jnp.arange(2 * BLOCK)[None, :]
    key_sub = jnp.arange(nb)[:, None] * BLOCK - BLOCK + jnp.arange(2 * BLOCK)[None, :]
    mask = ((step >= 0) & (step <= span))[None] & (key_sub >= 0)[:, None, :]
    logits = logits - slopes[None, :, None, None, None] * (step * dilation).astype(jnp.float32)
    logits = jnp.where(mask[None, None], logits, -jnp.inf)
    lse = jax.nn.logsumexp(logits, axis=-1)
    p = jnp.exp(logits - lse[..., None]).astype(v.dtype)
    o = jnp.einsum('zhnqk,znkhd->znqhd', p, vv)

    def from_sub(a):
        tail = a.shape[4:]
        a = a.reshape((B, dilation, n_pad, h) + tail)
        a = jnp.swapaxes(a, 1, 2).reshape((B, s_pad, h) + tail)
        return a[:, :S]

    return from_sub(o), from_sub(jnp.moveaxis(lse, 1, -1))


def hybrid_mixer(x, w_in, w_a, w_b, w_out, slopes):
    B, S, _ = x.shape
    split_points = [int(c) for c in np.cumsum(IN_SIZES)[:-1]]
    proj = x @ w_in
    aq, ak, av, iq, ik, iw, bq, bk, bv, ga, gb = jnp.split(proj, split_points, axis=-1)

    def heads(a, n):
        return a.reshape(B, S, n, -1)

    o_a = dsa_attention(heads(aq, N_SPARSE_HEADS), heads(ak, N_SPARSE_HEADS), heads(av, N_SPARSE_HEADS),
                        heads(iq, N_IDX_HEADS), ik, iw * N_IDX_HEADS ** -0.5,
                        slopes[N_DIL_HEADS:])

    bq4, bk4, bv4 = heads(bq, N_DIL_HEADS), heads(bk, N_DIL_HEADS), heads(bv, N_DIL_HEADS)
    outs, lses = [], []
    for g, (win, dil) in enumerate(DIL_GROUPS):
        sl = slice(g * HEADS_PER_DIL_GROUP, (g + 1) * HEADS_PER_DIL_GROUP)
        o_g, l_g = dilated_group_attention(bq4[:, :, sl], bk4[:, :, sl], bv4[:, :, sl], win, dil, slopes[sl])
        outs.append(o_g)
        lses.append(l_g)
    wts = jax.nn.softmax(jnp.stack(lses), axis=0).astype(x.dtype)
    o_b = jnp.einsum('gbsh,gbshd->bshd', wts, jnp.stack(outs)).reshape(B, S, DIL_OUT_WIDTH)

    merged = jax.nn.sigmoid(ga) * (o_a @ w_a) + jax.nn.sigmoid(gb) * (o_b @ w_b)
    return merged @ w_out


def swiglu_ffn(h, w_gate, w_up, w_down):
    return (jax.nn.silu(h @ w_gate) * (h @ w_up)) @ w_down


def setup_inputs(seed: int = 0) -> dict:
    key = jax.random.key(seed)
    ks = jax.random.split(key, 12)
    f32 = jnp.float32
    x = jax.random.normal(ks[0], (BATCH, SEQ, D_MODEL), f32)
    col_scale = jnp.concatenate([
        jnp.full((n,), DEEPNORM_BETA if i in V_COLUMN_SLOTS else 1.0, f32)
        for i, n in enumerate(IN_SIZES)])
    w_in = jax.random.normal(ks[1], (DEPTH, D_MODEL, IN_WIDTH), f32) * (D_MODEL ** -0.5) * col_scale
    w_a = jax.random.normal(ks[2], (DEPTH, SPARSE_WIDTH, D_MODEL), f32) * (SPARSE_WIDTH ** -0.5 * DEEPNORM_BETA)
    w_b = jax.random.normal(ks[3], (DEPTH, DIL_OUT_WIDTH, D_MODEL), f32) * (DIL_OUT_WIDTH ** -0.5 * DEEPNORM_BETA)
    w_out = jax.random.normal(ks[4], (DEPTH, D_MODEL, D_MODEL), f32) * (D_MODEL ** -0.5 * DEEPNORM_BETA)
    ln1_g = 1.0 + 0.01 * jax.random.normal(ks[5], (DEPTH, D_MODEL), f32)
    ln1_b = 0.01 * jax.random.normal(ks[6], (DEPTH, D_MODEL), f32)
    w_gate = jax.random.normal(ks[7], (DEPTH, D_MODEL, D_FF), f32) * (D_MODEL ** -0.5)
    w_up = jax.random.normal(ks[8], (DEPTH, D_MODEL, D_FF), f32) * (D_MODEL ** -0.5 * DEEPNORM_BETA)
    w_down = jax.random.normal(ks[9], (DEPTH, D_FF, D_MODEL), f32) * (D_FF ** -0.5 * DEEPNORM_BETA)
    ln2_g = 1.0 + 0.01 * jax.random.normal(ks[10], (DEPTH, D_MODEL), f32)
    ln2_b = 0.01 * jax.random.normal(ks[11], (DEPTH, D_MODEL), f32)
    return {"x": x, "w_in": w_in, "w_a": w_a, "w_b": w_b, "w_out": w_out,
            "ln1_g": ln1_g, "ln1_b": ln1_b, "w_gate": w_gate, "w_up": w_up,
            "w_down": w_down, "ln2_g": ln2_g, "ln2_b": ln2_b}


def reference(x, w_in, w_a, w_b, w_out, ln1_g, ln1_b, w_gate, w_up, w_down, ln2_g, ln2_b):
    slopes = alibi_slopes()
    for l in range(DEPTH):
        h = layer_norm(DEEPNORM_ALPHA * x + hybrid_mixer(x, w_in[l], w_a[l], w_b[l], w_out[l], slopes),
                       ln1_g[l], ln1_b[l])
        x = layer_norm(DEEPNORM_ALPHA * h + swiglu_ffn(h, w_gate[l], w_up[l], w_down[l]),
                       ln2_g[l], ln2_b[l])
    return x
```

```python
import math
from contextlib import ExitStack

import numpy as np
import ml_dtypes

import concourse.bass as bass
import concourse.mybir as mybir
from concourse.bass_utils import run_bass_kernel_spmd

F32 = mybir.dt.float32
BF16 = mybir.dt.bfloat16
I32 = mybir.dt.int32
AF = mybir.ActivationFunctionType
ALU = mybir.AluOpType

NCORES = 8
D = 2048
SEQ = 8192
T = 1024
NLB = 8
KC = 16
DFF = 5632
FC = DFF // 128
ALPHA = 2.0 ** 0.25
EPS = 1e-5
TOPK = 256
NBIS = 18
BIS_LO, BIS_W = -16.0, 32.0
SCALE = 128.0 ** -0.5
NEG = -30000.0
DIL = ((128, 1), (512, 4), (2048, 16))
LBACK = (1, 4, 16)

IN_OFF = np.cumsum([0, 1024, 1024, 1024, 1024, 64, 16, 1536, 1536, 1536, 2048, 2048])
C_AQ, C_AK, C_AV, C_IQ, C_IK, C_IW, C_BQ, C_BK, C_BV, C_GA, C_GB = [int(v) for v in IN_OFF[:11]]
KV_AK, KV_BK, KV_IK, KV_AV, KV_BV, KV_W = 0, 1024, 2560, 2624, 3648, 5184
Q_AQ, Q_IQ, Q_BQ, Q_IW, Q_GA, Q_GB, Q_W = 0, 1024, 2048, 3584, 3600, 5648, 7696
KROWS = 2624
VCOLS = 2560
QROWS = 3584


def blocks_of(c):
    return [16 * (lb // 2) + (c if lb % 2 == 0 else 15 - c) for lb in range(NLB)]


def kb_of(lb, r):
    return 8 * lb + (r if lb % 2 == 0 else 7 - r)


def lbr_of(kb):
    lb = kb // 8
    o = kb % 8
    return lb, (o if lb % 2 == 0 else 7 - o)


def alibi_slopes():
    return np.exp2(-8.0 * np.arange(1, 21, dtype=np.float64) / 20.0)


class Prog:
    ENGS = ("pe", "act", "dve", "pool", "sp")
    EPOCH = 24000

    def __init__(self, nc, stack):
        self.nc = nc
        self.stack = stack
        self.sems = {}
        self.ops = {e: [] for e in self.ENGS}
        self.cnt = {e: 0 for e in self.ENGS}
        self.epoch = {e: 0 for e in self.ENGS}
        self.acount = {}
        self.lastw = {}
        self.readers = {}
        self.waited = {e: {} for e in self.ENGS}
        self.floor = {}
        self.nsem = 0

    def _sem(self, name):
        if name not in self.sems:
            self.nsem += 1
            assert self.nsem <= 100, "too many semaphores"
            self.sems[name] = self.stack.enter_context(self.nc.semaphore(name.replace("|", "_")))
        return name

    def _esem(self, e):
        return self._sem(f"es_{e}_{self.epoch[e]}")

    def _waits_for(self, e, reads, writes):
        need = {}

        def add(ev):
            if ev is None:
                return
            s, v = ev
            if v > need.get(s, 0):
                need[s] = v

        for k in reads:
            add(self.lastw.get(k))
        for k in writes:
            add(self.lastw.get(k))
            for s, v in self.readers.get(k, {}).items():
                add((s, v))
        out = []
        for s, v in need.items():
            if e == "pe" and s.startswith("es_pe_"):
                continue
            if v <= self.floor.get(s, 0) or v <= self.waited[e].get(s, 0):
                continue
            self.waited[e][s] = v
            out.append((s, v))
        return out

    def _commit(self, ev, reads, writes):
        for k in writes:
            self.lastw[k] = ev
            self.readers[k] = {}
        for k in reads:
            d = self.readers.setdefault(k, {})
            if ev[1] > d.get(ev[0], 0):
                d[ev[0]] = ev[1]

    def op(self, e, fn, r=(), w=()):
        waits = self._waits_for(e, r, w)
        if self.cnt[e] >= self.EPOCH:
            self.epoch[e] += 1
            self.cnt[e] = 0
        s = self._esem(e)
        self.cnt[e] += 1
        ev = (s, self.cnt[e])
        self.ops[e].append((waits, fn, (s, 1)))
        self._commit(ev, r, w)

    def dma(self, e, fn, semkey, r=(), w=(), n=1, amt=16):
        waits = self._waits_for(e, r, w)
        s = self._sem("as_" + semkey)
        self.acount[s] = self.acount.get(s, 0) + amt * n
        ev = (s, self.acount[s])
        self.ops[e].append((waits, fn, (s, amt)))
        self._commit(ev, r, w)

    def wait_all(self, e="sp"):
        waits = []
        for s, v in self.acount.items():
            if v > self.floor.get(s, 0) and v > self.waited[e].get(s, 0):
                waits.append((s, v))
                self.waited[e][s] = v
        self.ops[e].append((waits, None, None))

    def wait_keys(self, e, keys):
        waits = self._waits_for(e, keys, keys)
        self.ops[e].append((waits, None, None))

    def run_block(self):
        nc = self.nc
        sems = self.sems
        ops = self.ops

        def emit(e, eng):
            for waits, fn, inc in ops[e]:
                for s, v in waits:
                    eng.wait_ge(sems[s], v)
                if fn is None:
                    continue
                res = fn(eng)
                if inc is not None:
                    if not isinstance(res, (list, tuple)):
                        res = [res]
                    for ins in res:
                        ins.then_inc(sems[inc[0]], inc[1])

        with nc.Block() as block:
            @block.tensor
            def _(eng):
                emit("pe", eng)

            @block.scalar
            def _(eng):
                emit("act", eng)

            @block.vector
            def _(eng):
                emit("dve", eng)

            @block.gpsimd
            def _(eng):
                emit("pool", eng)

            @block.sync
            def _(eng):
                emit("sp", eng)

        for e in self.ENGS:
            self.floor[f"es_{e}_{self.epoch[e]}"] = self.cnt[e]
            for ep in range(self.epoch[e]):
                self.floor[f"es_{e}_{ep}"] = self.EPOCH + 1
        for e in self.ENGS:
            for s, v in self.waited[e].items():
                if s.startswith("as_") and v > self.floor.get(s, 0):
                    self.floor[s] = v
        self.ops = {e: [] for e in self.ENGS}


def build_nc(debug=None):
    nc = bass.Bass("TRN2", target_bir_lowering=False)

    def din(name, shape, dt=F32):
        return nc.dram_tensor(name, list(shape), dt, kind="ExternalInput").ap()

    xT_d = din("xT", [D, T])
    xtok_d = din("xtok", [T, D])
    wkv_d = din("wkv", [256, KV_W])
    wq_d = din("wq", [256, Q_W])
    wa_d = din("wa", [128, D])
    wb_d = din("wb", [64, D])
    wo_d = din("wo", [256, D])
    wg_d = din("wg", [256, DFF])
    wu_d = din("wu", [256, DFF])
    wd_d = din("wd", [704, D])
    lnp_d = din("lnp", [128, 4, D])
    ident_d = din("ident", [128, 128])
    sbias_d = din("sbias", [128, 64 * 8])
    dbias_d = din("dbias", [128, 24 * 12])
    a1_d = din("a1", [128, 12 * 128])
    b1_d = din("b1", [128, 8 * 128])
    posp_d = din("posp", [128, 16])
    posrow_d = din("posrow", [128, 8 * 128])
    krel_d = din("krel", [128, 2 * 1024])
    out_d = nc.dram_tensor("out", [T, D], F32, kind="ExternalOutput").ap()
    dbg_d = None
    if debug is not None:
        dbg_d = nc.dram_tensor("dbg", list(debug[1]), F32, kind="ExternalOutput").ap()

    def dint(name, shape, dt=BF16):
        return nc.dram_tensor(name, list(shape), dt)

    WKV_l, WKV = dint("WKV_l", [256, KV_W]), dint("WKV", [2048, KV_W])
    WQ_l, WQ = dint("WQ_l", [256, Q_W]), dint("WQ", [2048, Q_W])
    WA_l, WA = dint("WA_l", [128, D]), dint("WA", [1024, D])
    WB_l, WB = dint("WB_l", [64, D]), dint("WB", [512, D])
    WO_l, WO = dint("WO_l", [256, D]), dint("WO", [2048, D])
    WG_l, WG = dint("WG_l", [256, DFF]), dint("WG", [2048, DFF])
    WU_l, WU = dint("WU_l", [256, DFF]), dint("WU", [2048, DFF])
    WD_l, WD = dint("WD_l", [704, D]), dint("WD", [DFF, D])
    kloc, KALL = dint("kloc", [KROWS, T]), dint("KALL", [8 * KROWS, T])
    vloc, VALL = dint("vloc", [T, VCOLS]), dint("VALL", [8 * T, VCOLS])
    qloc = dint("qloc", [QROWS, T])
    hres = dint("hres", [T, D], F32)

    stack = ExitStack()
    with stack:
        P = Prog(nc, stack)
        sb = lambda name, shape, dt=BF16: nc.sbuf_tensor("s_" + name, list(shape), dt)
        PS = [stack.enter_context(nc.psum_tensor(f"ps{i}", [128, 512], F32)) for i in range(7)]
        PST = stack.enter_context(nc.psum_tensor("pst", [128, 1024], BF16))
        bank_ctr = [0]

        def nbank():
            b = bank_ctr[0] % 7
            bank_ctr[0] += 1
            return b

        ident = stack.enter_context(sb("ident", [128, 128]))
        oaT = stack.enter_context(sb("oaT", [128, 8, T]))
        obT = stack.enter_context(sb("obT", [128, 4, T]))
        iw_sb = stack.enter_context(sb("iw", [128, NLB, 16], F32))
        posp = stack.enter_context(sb("posp", [128, 16], F32))
        evac_ctr = [0]

        def evac(out_ap, in_ap, r, w):
            evac_ctr[0] += 1
            if evac_ctr[0] % 2:
                P.op("act", lambda e, o=out_ap, i=in_ap: e.copy(out=o, in_=i), r=r, w=w)
            else:
                P.op("dve", lambda e, o=out_ap, i=in_ap: e.tensor_copy(out=o, in_=i), r=r, w=w)

        def dump_and_stop(aps, dram_aps=(), stage=None):
            with ExitStack() as ph:
                tf = stage if stage is not None else ph.enter_context(sb("dumpf", [128, 1024], F32))
                for i, a in enumerate(aps):
                    P.op("dve", lambda e, a=a: e.tensor_copy(out=tf[:, 0:a.shape[-1]], in_=a), w=("dumpf",))
                    P.dma("sp", lambda e, i=i, a=a: e.dma_start(out=dbg_d[i * 128:(i + 1) * 128, 0:a.shape[-1]], in_=tf[:, 0:a.shape[-1]], allow_slow_non_contiguous=True),
                          "dumpf_st", r=("dumpf",), w=(f"dbg{i}",))
                for i, (dst, src) in enumerate(dram_aps):
                    P.dma("sp", lambda e, dst=dst, src=src: e.dma_start(out=dst, in_=src), "dumpd", w=(f"dbgd{i}",))
                P.wait_all("sp")
                P.run_block()

        def cast_ag(src_d, loc, full, nrows, key):
            step = 128 if nrows % 128 == 0 else 64
            assert nrows % step == 0
            n = nrows // step

            def f(e, src_d=src_d, loc=loc):
                res = []
                for i in range(n):
                    res.append(e.dma_start(out=loc.ap()[i * step:(i + 1) * step, :],
                                           in_=src_d[i * step:(i + 1) * step, :], max_dma_last_dim=4096))
                return res
            P.dma("pool", f, key + "_c", r=(), w=(key + "_l",), n=n)
            P.dma("pool", lambda e, loc=loc, full=full: e.collective_compute(
                "AllGather", ALU.bypass, replica_groups=[list(range(NCORES))],
                ins=[loc.ap().opt()], outs=[full.ap().opt()]), key + "_g", r=(key + "_l",), w=(key,), amt=1)

        with ExitStack() as ph:
            xT = ph.enter_context(sb("xT", [128, KC, T]))
            WS = [ph.enter_context(sb(f"ws{i}", [128, KC, 512])) for i in range(3)]
            STG = [ph.enter_context(sb(f"stg{i}", [128, T])) for i in range(4)]
            identf = ph.enter_context(sb("identf", [128, 128], F32))

            P.dma("sp", lambda e: e.dma_start(out=identf[:], in_=ident_d), "identf", w=("identf",))
            P.op("dve", lambda e: e.tensor_copy(out=ident[:], in_=identf[:]), r=("identf",), w=("ident",))
            P.dma("sp", lambda e: e.dma_start(out=posp[:], in_=posp_d), "posp", w=("posp",))

            def ld_xT(e):
                res = []
                src = xT_d.rearrange("(kc p) t -> p kc t", p=128)
                for i in range(4):
                    res.append(e.dma_start(out=xT[:, 4 * i:4 * i + 4, :], in_=src[:, 4 * i:4 * i + 4, :]))
                return res
            P.dma("pool", ld_xT, "xT", w=("xT",), n=4)
            cast_ag(wkv_d, WKV_l, WKV, 256, "WKV")
            cast_ag(wq_d, WQ_l, WQ, 256, "WQ")

            ws_ctr = [0]

            def load_w(Wd, key, c0, w):
                slot = ws_ctr[0] % 3
                ws_ctr[0] += 1
                src = Wd.ap()[:, c0:c0 + w].rearrange("(kc p) n -> p kc n", p=128)
                P.dma("sp", lambda e, slot=slot, src=src, w=w: e.dma_start(out=WS[slot][:, :, 0:w], in_=src),
                      f"ws{slot}", r=(key,), w=(f"ws{slot}",))
                return slot

            stg_ctr = [0]
            store_keys = {"kloc": [], "vloc": [], "qloc": []}

            def proj_feat(Wd, key, c0, w, dst, dstname, row0):
                slot = load_w(Wd, key, c0, w)
                for fc in range((w + 127) // 128):
                    m = min(128, w - fc * 128)
                    sg = stg_ctr[0] % 4
                    stg_ctr[0] += 1
                    for half in range(2):
                        b = nbank()
                        for kc in range(KC):
                            P.op("pe", lambda e, b=b, slot=slot, kc=kc, fc=fc, m=m, half=half: e.matmul(
                                PS[b][0:m, :], lhsT=WS[slot][:, kc, fc * 128:fc * 128 + m],
                                rhs=xT[:, kc, half * 512:(half + 1) * 512], start=(kc == 0), stop=(kc == KC - 1)),
                                r=(f"ws{slot}", "xT"), w=(f"ps{b}",))
                        evac(STG[sg][0:m, half * 512:(half + 1) * 512], PS[b][0:m, :], r=(f"ps{b}",), w=(f"stg{sg}",))
                    k = f"{dstname}_{row0 + fc * 128}"
                    store_keys[dstname].append(k)
                    P.dma("sp", lambda e, sg=sg, m=m, r0=row0 + fc * 128: e.dma_start(
                        out=dst.ap()[r0:r0 + m, :], in_=STG[sg][0:m, :]), f"stg{sg}_st", r=(f"stg{sg}",), w=(k,))

            def proj_tok(Wd, key, c0, w, dst, dstname, col0):
                slot = load_w(Wd, key, c0, w)
                for lb in range(NLB):
                    b = nbank()
                    sg = stg_ctr[0] % 4
                    stg_ctr[0] += 1
                    for kc in range(KC):
                        P.op("pe", lambda e, b=b, slot=slot, kc=kc, lb=lb, w=w: e.matmul(
                            PS[b][:, 0:w], lhsT=xT[:, kc, lb * 128:(lb + 1) * 128], rhs=WS[slot][:, kc, 0:w],
                            start=(kc == 0), stop=(kc == KC - 1)), r=(f"ws{slot}", "xT"), w=(f"ps{b}",))
                    if dst is None:
                        P.op("act", lambda e, b=b, lb=lb: e.mul(out=iw_sb[:, lb, :], in_=PS[b][:, 0:16], mul=1.0 / 32.0),
                             r=(f"ps{b}",), w=("iw",))
                        continue
                    evac(STG[sg][:, 0:w], PS[b][:, 0:w], r=(f"ps{b}",), w=(f"stg{sg}",))
                    k = f"{dstname}_{lb}_{col0}"
                    store_keys[dstname].append(k)
                    P.dma("sp", lambda e, sg=sg, lb=lb, col0=col0, w=w: e.dma_start(
                        out=dst.ap()[lb * 128:(lb + 1) * 128, col0:col0 + w], in_=STG[sg][:, 0:w]),
                        f"stg{sg}_st", r=(f"stg{sg}",), w=(k,))

            for i in range(2):
                proj_feat(WKV, "WKV", KV_AK + 512 * i, 512, kloc, "kloc", 512 * i)
            for i in range(3):
                proj_feat(WKV, "WKV", KV_BK + 512 * i, 512, kloc, "kloc", 1024 + 512 * i)
            proj_feat(WKV, "WKV", KV_IK, 64, kloc, "kloc", 2560)
            for i in range(2):
                proj_tok(WKV, "WKV", KV_AV + 512 * i, 512, vloc, "vloc", 512 * i)
            for i in range(3):
                proj_tok(WKV, "WKV", KV_BV + 512 * i, 512, vloc, "vloc", 1024 + 512 * i)
            P.dma("pool", lambda e: e.collective_compute(
                "AllGather", ALU.bypass, replica_groups=[list(range(NCORES))],
                ins=[kloc.ap().opt()], outs=[KALL.ap().opt()]), "KALL_g", r=tuple(store_keys["kloc"]), w=("KALL",), amt=1)
            P.dma("pool", lambda e: e.collective_compute(
                "AllGather", ALU.bypass, replica_groups=[list(range(NCORES))],
                ins=[vloc.ap().opt()], outs=[VALL.ap().opt()]), "VALL_g", r=tuple(store_keys["vloc"]), w=("VALL",), amt=1)
            cast_ag(wa_d, WA_l, WA, 128, "WA")
            cast_ag(wb_d, WB_l, WB, 64, "WB")
            cast_ag(wo_d, WO_l, WO, 256, "WO")
            cast_ag(wg_d, WG_l, WG, 256, "WG")
            cast_ag(wu_d, WU_l, WU, 256, "WU")
            cast_ag(wd_d, WD_l, WD, 704, "WD")
            for i in range(2):
                proj_feat(WQ, "WQ", Q_AQ + 512 * i, 512, qloc, "qloc", 512 * i)
            for i in range(2):
                proj_feat(WQ, "WQ", Q_IQ + 512 * i, 512, qloc, "qloc", 1024 + 512 * i)
            for i in range(3):
                proj_feat(WQ, "WQ", Q_BQ + 512 * i, 512, qloc, "qloc", 2048 + 512 * i)
            proj_tok(WQ, "WQ", Q_IW, 16, None, "iw", 0)
            P.wait_keys("sp", tuple(store_keys["qloc"]) + tuple(store_keys["kloc"]) + tuple(store_keys["vloc"]) + ("KALL", "VALL"))
            for k in store_keys["qloc"]:
                P.lastw[k] = None
            P.lastw["qloc"] = None
            P.run_block()

        if debug is not None and debug[0] == "p1":
            with ExitStack() as ph:
                t = ph.enter_context(sb("dbgt", [128, 1024]))
                tf = ph.enter_context(sb("dbgf", [128, 1024], F32))
                srcs = [KALL.ap()[0:128, :], KALL.ap()[KROWS * 3 + 2560:KROWS * 3 + 2560 + 128, :],
                        VALL.ap()[1024 * 5:1024 * 5 + 128, 0:1024], qloc.ap()[1024:1152, :]]
                for i, s_ in enumerate(srcs):
                    P.dma("sp", lambda e, s_=s_: e.dma_start(out=t[:], in_=s_), "dbgt", w=("dbgt",))
                    P.op("dve", lambda e: e.tensor_copy(out=tf[:], in_=t[:]), r=("dbgt",), w=("dbgf",))
                    P.dma("sp", lambda e, i=i: e.dma_start(out=dbg_d[i * 128:(i + 1) * 128, :], in_=tf[:]), "dbgf_st", r=("dbgf",), w=(f"dbg{i}",))
                P.op("dve", lambda e: e.tensor_copy(out=tf[:, 0:128], in_=iw_sb[:].rearrange("p a b -> p (a b)")), r=("iw",), w=("dbgf",))
                P.dma("sp", lambda e: e.dma_start(out=dbg_d[512:640, 0:128], in_=tf[:, 0:128]), "dbgf_st", r=("dbgf",), w=("dbg9",))
                P.dma("sp", lambda e: e.dma_start(out=out_d[0:128, 0:1024], in_=tf[:]), "dbgf_st", r=("dbgf",), w=("o",))
                P.wait_all("sp")
                P.run_block()
            return nc


        def normalize_head(b, dst_ap, tagr):
            P.op("dve", lambda e, b=b: e.reciprocal(out=rec[:], in_=PS[b][:, 128:129]), r=(f"ps{b}",), w=("rec",))
            P.op("dve", lambda e, b=b, d=dst_ap: e.tensor_scalar(out=d, in0=PS[b][:, 0:128], scalar1=rec[:], scalar2=None,
                                                                 op0=ALU.mult), r=(f"ps{b}", "rec"), w=(tagr,))

        qk_ctr = [0]
        pt_ctr = [0]

        def attn_blocks(blocks, acc_b, first_last, KT_ap_fn, q_ap, mb_ap_fn, v_ap_fn, bias_ap_fn, rkeys, extra=None):
            nb = len(blocks)
            for n, blk in enumerate(blocks):
                qi = qk_ctr[0]
                qk_ctr[0] += 1
                b, sub = (qi // 4) % 2, qi % 4
                pk = f"ps{b}_{sub}"
                dst = PS[b][:, sub * 128:(sub + 1) * 128]
                P.op("pe", lambda e, dst=dst, l=KT_ap_fn(blk), q=q_ap: e.matmul(dst, lhsT=l, rhs=q, start=True, stop=False),
                     r=rkeys, w=(pk,))
                P.op("pe", lambda e, dst=dst, m=mb_ap_fn(blk, n): e.matmul(dst, lhsT=ident[:], rhs=m, start=False, stop=(extra is None)),
                     r=rkeys + ("ident",), w=(pk,))
                if extra is not None:
                    P.op("pe", lambda e, dst=dst, x=extra: e.matmul(dst, lhsT=x[0], rhs=x[1], start=False, stop=True),
                         r=rkeys + ("a1", "b1"), w=(pk,))
                k = pt_ctr[0] % 4
                pt_ctr[0] += 1
                P.op("act", lambda e, k=k, dst=dst, bi=bias_ap_fn(blk): e.activation(out=pT[k][:], in_=dst, func=AF.Exp, bias=bi, scale=SCALE),
                     r=(pk, "biastab"), w=(f"pT{k}",))
                fl_first = first_last[0] and n == 0
                fl_last = first_last[1] and n == nb - 1
                P.op("pe", lambda e, k=k, v=v_ap_fn(blk), f=fl_first, l=fl_last: e.matmul(PS[acc_b][:, 0:129], lhsT=pT[k][:], rhs=v, start=f, stop=l),
                     r=rkeys + (f"pT{k}",), w=(f"ps{acc_b}",))

        with ExitStack() as ph:
            bq = [ph.enter_context(sb(f"bq{i}", [128, 12, 128])) for i in range(2)]
            KTd = [ph.enter_context(sb(f"ktd{i}", [128, 8, 384])) for i in range(2)]
            Vd = [ph.enter_context(sb(f"vd{i}", [128, 8, 3, 129])) for i in range(2)]
            MBD = [ph.enter_context(sb(f"mbd{i}", [128, 24, 128])) for i in range(3)]
            posrow = ph.enter_context(sb("posrow", [128, 8 * 128], F32))
            dbias = ph.enter_context(sb("dbias", [128, 24 * 12], F32))
            stgf = ph.enter_context(sb("stgf", [128, 12 * 128], F32))
            a1 = ph.enter_context(sb("a1", [128, 12 * 128]))
            b1 = ph.enter_context(sb("b1", [128, 8 * 128]))
            pT = [ph.enter_context(sb(f"pT{i}", [128, 128])) for i in range(4)]
            obj = [ph.enter_context(sb(f"obj{i}", [128, 512])) for i in range(2)]
            rec = ph.enter_context(sb("rec", [128, 1], F32))
            Dt = ph.enter_context(sb("Dt", [128, 128], F32))
            At = ph.enter_context(sb("At", [128, 128], F32))
            Bt = ph.enter_context(sb("Bt", [128, 128], F32))
            Ct = ph.enter_context(sb("Ct", [128, 128], F32))
            Di = ph.enter_context(sb("Di", [128, 128], I32))
            Ri = ph.enter_context(sb("Ri", [128, 128], I32))

            P.dma("sp", lambda e: e.dma_start(out=posrow[:], in_=posrow_d), "posrow", w=("posrow",))
            P.dma("sp", lambda e: e.dma_start(out=dbias[:], in_=dbias_d), "biastab", w=("biastab",))
            P.dma("sp", lambda e: e.dma_start(out=stgf[:], in_=a1_d), "stgf", w=("stgf",))
            P.op("dve", lambda e: e.tensor_copy(out=a1[:], in_=stgf[:]), r=("stgf",), w=("a1",))
            P.dma("sp", lambda e: e.dma_start(out=stgf[:, 0:1024], in_=b1_d), "stgf", w=("stgf",))
            P.op("dve", lambda e: e.tensor_copy(out=b1[:], in_=stgf[:, 0:1024]), r=("stgf",), w=("b1",))
            for i in range(2):
                P.op("pool", lambda e, i=i: e.memset(Vd[i][:, :, :, 128:129], 1.0), w=(f"vd{i}",))
            pcol = posp[:, 8:9]
            kv_ctr = 0
            for j in range(NLB):
                jq = j % 2
                P.dma("sp", lambda e, j=j, jq=jq: e.dma_start(
                    out=bq[jq][:], in_=qloc.ap()[2048:3584, j * 128:(j + 1) * 128].rearrange("(h d) t -> d h t", d=128)),
                    f"bq{jq}", r=("qloc",), w=(f"bq{jq}",))
                blist = []
                for g in range(3):
                    win, dil = DIL[g]
                    kbs = list(range(max(0, 8 * j - LBACK[g]), 8 * j + 8))
                    blist.append(kbs)
                    for n, kb in enumerate(kbs):
                        P.op("dve", lambda e, j=j, kb=kb: e.tensor_scalar(out=Dt[:], in0=posrow[:, j * 128:(j + 1) * 128], scalar1=pcol,
                                                                        scalar2=float(128 * kb), op0=ALU.subtract, op1=ALU.subtract),
                             r=("posrow", "posp"), w=("Dt",))
                        P.op("dve", lambda e: e.tensor_scalar(out=At[:], in0=Dt[:], scalar1=0.0, scalar2=None, op0=ALU.is_ge), r=("Dt",), w=("At",))
                        P.op("dve", lambda e, win=win: e.scalar_tensor_tensor(out=Bt[:], in0=Dt[:], scalar=float(win), in1=At[:], op0=ALU.is_le, op1=ALU.mult),
                             r=("Dt", "At"), w=("Bt",))
                        src = "Bt"
                        srct = Bt
                        if dil > 1:
                            P.op("dve", lambda e: e.tensor_copy(out=Di[:], in_=Dt[:]), r=("Dt",), w=("Di",))
                            P.op("dve", lambda e, dil=dil: e.tensor_single_scalar(out=Ri[:], in_=Di[:], scalar=dil - 1, op=ALU.bitwise_and), r=("Di",), w=("Ri",))
                            P.op("dve", lambda e: e.scalar_tensor_tensor(out=Ct[:], in0=Ri[:], scalar=0.0, in1=Bt[:], op0=ALU.is_equal, op1=ALU.mult),
                                 r=("Ri", "Bt"), w=("Ct",))
                            src, srct = "Ct", Ct
                        P.op("dve", lambda e, g=g, n=n, srct=srct: e.tensor_scalar(out=MBD[g][:, n, :], in0=srct[:], scalar1=1.0, scalar2=-NEG,
                                                                                  op0=ALU.subtract, op1=ALU.mult), r=(src,), w=(f"mbd{g}",))
                for hh in range(4):
                    acc_b = 3 + (hh % 2)
                    for g in range(3):
                        gh = 4 * g + hh
                        kbs = blist[g]
                        lb0 = kbs[0] // 8
                        nlb = j + 1 - lb0
                        s_ = kv_ctr % 2
                        kv_ctr += 1
                        ksrc = KALL.ap().rearrange("(r f) t -> f r t", r=8)[1024 + gh * 128:1024 + (gh + 1) * 128, :, lb0 * 128:(j + 1) * 128]
                        P.dma("sp", lambda e, s_=s_, ksrc=ksrc, nlb=nlb: e.dma_start(out=KTd[s_][:, :, 0:nlb * 128], in_=ksrc),
                              f"ktd{s_}", r=("KALL",), w=(f"ktd{s_}",))
                        vsrc = VALL.ap().rearrange("(r lb p) c -> p r lb c", r=8, lb=8)

                        def ldv(e, s_=s_, vsrc=vsrc, gh=gh, lb0=lb0, j=j, nlb=nlb):
                            return [e.dma_start(out=Vd[s_][:, r, 0:nlb, 0:128],
                                                in_=vsrc[:, r, lb0:j + 1, 1024 + gh * 128:1024 + (gh + 1) * 128]) for r in range(8)]
                        P.dma("act", ldv, f"vd{s_}", r=("VALL",), w=(f"vd{s_}",), n=8)
                        blocks = [(kb,) + lbr_of(kb) for kb in kbs]
                        attn_blocks(
                            blocks, acc_b, (g == 0, g == 2),
                            lambda blk, s_=s_, lb0=lb0: KTd[s_][:, blk[2], (blk[1] - lb0) * 128:(blk[1] - lb0 + 1) * 128],
                            bq[jq][:, gh, :],
                            lambda blk, n, g=g: MBD[g][:, n, :],
                            lambda blk, s_=s_, lb0=lb0: Vd[s_][:, blk[2], blk[1] - lb0, :],
                            lambda blk, gh=gh, j=j: dbias[:, (blk[0] - (8 * j + 8) + 24) * 12 + gh:(blk[0] - (8 * j + 8) + 24) * 12 + gh + 1],
                            (f"ktd{s_}", f"vd{s_}", f"bq{jq}", f"mbd{g}"),
                            extra=(a1[:, gh * 128:(gh + 1) * 128], b1[:, j * 128:(j + 1) * 128]))
                    normalize_head(acc_b, obj[jq][:, hh * 128:(hh + 1) * 128], f"obj{jq}")
                for i in range(4):
                    P.op("pe", lambda e, i=i, jq=jq: e.transpose(PST[:, i * 128:(i + 1) * 128], obj[jq][:, i * 128:(i + 1) * 128], ident[:]),
                         r=(f"obj{jq}", "ident"), w=("pst",))
                P.op("act", lambda e, j=j: e.copy(out=obT[:, :, j * 128:(j + 1) * 128], in_=PST[:, 0:512].rearrange("p (a b) -> p a b", b=128)),
                     r=("pst",), w=("obT",))
            P.wait_all("sp")
            P.run_block()
            if debug is not None and debug[0] == "p3b":
                dump_and_stop([obT[:, i, :] for i in range(4)])
                return nc

        with ExitStack() as ph:
            kid = [ph.enter_context(sb(f"kid{i}", [128, 8, T])) for i in range(2)]
            Isc = ph.enter_context(sb("Isc", [128, NLB, 8, 128], F32))
            MBt = [ph.enter_context(sb(f"mbt{i}", [128, 1024])) for i in range(2)]
            MBT = ph.enter_context(sb("MBT", [128, 64, 128]))
            KT = [ph.enter_context(sb(f"kt{i}", [128, 8, T])) for i in range(2)]
            Vs = [ph.enter_context(sb(f"vs{i}", [128, 8, NLB, 129])) for i in range(2)]
            iqj = [ph.enter_context(sb(f"iqj{i}", [128, 8, 128])) for i in range(2)]
            qj = [ph.enter_context(sb(f"qj{i}", [128, 8, 128])) for i in range(2)]
            Y = [ph.enter_context(sb(f"Y{i}", [128, 512])) for i in range(4)]
            pT = [ph.enter_context(sb(f"pTs{i}", [128, 128])) for i in range(4)]
            oaj = [ph.enter_context(sb(f"oaj{i}", [128, 1024])) for i in range(2)]
            rec = ph.enter_context(sb("recs", [128, 1], F32))
            sbias = ph.enter_context(sb("sbias", [128, 64 * 8], F32))
            krel = ph.enter_context(sb("krel", [128, 2 * 1024], F32))
            ctmp = ph.enter_context(sb("ctmp", [128, 1024], F32))
            junk = KT[1][:].rearrange("p a b -> p (a b)")
            mid = ph.enter_context(sb("mid", [128, 1], F32))
            cnt = ph.enter_context(sb("cnt", [128, 1], F32))
            stp = ph.enter_context(sb("stp", [128, 1], F32))
            theta = ph.enter_context(sb("theta", [128, 1], F32))

            P.dma("sp", lambda e: e.dma_start(out=sbias[:], in_=sbias_d), "biastab", w=("biastab",))
            P.dma("sp", lambda e: e.dma_start(out=krel[:], in_=krel_d), "krel", w=("krel",))
            for i in range(2):
                P.op("pool", lambda e, i=i: e.memset(kid[i][:], 0.0), w=(f"kid{i}",))
                P.op("pool", lambda e, i=i: e.memset(Vs[i][:, :, :, 128:129], 1.0), w=(f"vs{i}",))
            iksrc = KALL.ap().rearrange("(r f) t -> f r t", r=8)[2560:2624, :, :]
            P.dma("sp", lambda e: e.dma_start(out=kid[0][0:64, :, :], in_=iksrc), "kid0", r=("KALL",), w=("kid0",))
            P.dma("sp", lambda e: e.dma_start(out=kid[1][64:128, :, :], in_=iksrc), "kid1", r=("KALL",), w=("kid1",))
            kv_ctr = 0
            y_ctr = 0
            for j in range(NLB):
                jq = j % 2
                nk = (j + 1) * 1024
                P.dma("sp", lambda e, j=j, jq=jq: e.dma_start(
                    out=iqj[jq][:], in_=qloc.ap()[1024:2048, j * 128:(j + 1) * 128].rearrange("(h d) t -> d h t", d=128)),
                    f"iqj{jq}", r=("qloc",), w=(f"iqj{jq}",))
                P.dma("sp", lambda e, j=j, jq=jq: e.dma_start(
                    out=qj[jq][:], in_=qloc.ap()[0:1024, j * 128:(j + 1) * 128].rearrange("(h d) t -> d h t", d=128)),
                    f"qj{jq}", r=("qloc",), w=(f"qj{jq}",))
                for lb in range(j + 1):
                    for rg in range(2):
                        for h in range(16):
                            rb = 5 + (y_ctr % 2)
                            ys = y_ctr % 4
                            y_ctr += 1
                            P.op("pe", lambda e, rb=rb, jq=jq, h=h, rg=rg, lb=lb: e.matmul(
                                PS[rb][:, :], lhsT=iqj[jq][:, h // 2, :], rhs=kid[h % 2][:, 4 * rg:4 * rg + 4, lb * 128:(lb + 1) * 128],
                                start=True, stop=True), r=(f"iqj{jq}", f"kid{h % 2}"), w=(f"ps{rb}",))
                            P.op("dve", lambda e, rb=rb, ys=ys, j=j, h=h: e.tensor_scalar(
                                out=Y[ys][:], in0=PS[rb][:, :], scalar1=0.0, scalar2=iw_sb[:, j, h:h + 1], op0=ALU.max, op1=ALU.mult),
                                r=(f"ps{rb}", "iw"), w=(f"Y{ys}",))
                            P.op("pe", lambda e, ys=ys, h=h: e.matmul(PS[2][:, :], lhsT=ident[:], rhs=Y[ys][:], start=(h == 0), stop=(h == 15)),
                                 r=(f"Y{ys}", "ident"), w=("ps2",))
                        P.op("act", lambda e, lb=lb, rg=rg: e.copy(out=Isc[:, lb, 4 * rg:4 * rg + 4, :], in_=PS[2][:, :].rearrange("p (a b) -> p a b", b=128)),
                             r=("ps2",), w=("Isc",))
                P.op("dve", lambda e, j=j: e.tensor_scalar(out=ctmp[:], in0=krel[:, (j % 2) * 1024:(j % 2 + 1) * 1024], scalar1=posp[:, j:j + 1],
                                                          scalar2=-1e30, op0=ALU.is_gt, op1=ALU.mult), r=("krel", "posp"), w=("ctmp",))
                P.op("dve", lambda e, j=j: e.tensor_tensor(out=Isc[:, j, :, :], in0=Isc[:, j, :, :], in1=ctmp[:].rearrange("p (a b) -> p a b", b=128), op=ALU.add),
                     r=("ctmp", "Isc"), w=("Isc",))
                Iflat = Isc[:, 0:j + 1, :, :].rearrange("p a b c -> p (a b c)")
                P.op("dve", lambda e: e.memset(mid[:], BIS_LO + BIS_W / 2), w=("mid",))
                wdt = BIS_W
                for it in range(NBIS):
                    P.op("dve", lambda e, nk=nk, Iflat=Iflat: e.tensor_scalar(out=junk[:, 0:nk], in0=Iflat, scalar1=mid[:], scalar2=None,
                                                                           op0=ALU.is_ge, op1=ALU.add, accum_out=cnt[:]),
                         r=("Isc", "mid"), w=("kt1", "cnt"))
                    P.op("dve", lambda e, wdt=wdt: e.tensor_scalar(out=stp[:], in0=cnt[:], scalar1=TOPK - 0.5, scalar2=wdt / 2, op0=ALU.is_ge, op1=ALU.mult),
                         r=("cnt",), w=("stp",))
                    P.op("dve", lambda e, wdt=wdt: e.scalar_tensor_tensor(out=mid[:], in0=stp[:], scalar=-wdt / 4, in1=mid[:], op0=ALU.add, op1=ALU.add),
                         r=("stp", "mid"), w=("mid",))
                    wdt = wdt / 2
                P.op("dve", lambda e, wdt=wdt: e.tensor_scalar(out=theta[:], in0=mid[:], scalar1=-wdt / 2, scalar2=None, op0=ALU.add), r=("mid",), w=("theta",))
                for lb in range(j + 1):
                    ms = lb % 2
                    P.op("dve", lambda e, lb=lb, ms=ms: e.tensor_scalar(out=MBt[ms][:], in0=Isc[:, lb, :, :].rearrange("p a b -> p (a b)"), scalar1=theta[:],
                                                                      scalar2=NEG, op0=ALU.is_lt, op1=ALU.mult), r=("Isc", "theta"), w=(f"mbt{ms}",))
                    for r in range(8):
                        P.op("pe", lambda e, ms=ms, r=r: e.transpose(PST[:, r * 128:(r + 1) * 128], MBt[ms][:, r * 128:(r + 1) * 128], ident[:]),
                             r=(f"mbt{ms}", "ident"), w=("pst",))
                    evac(MBT[:, lb * 8:(lb + 1) * 8, :], PST[:, :].rearrange("p (a b) -> p a b", b=128), r=("pst",), w=("MBT",))
                for h in range(8):
                    acc_b = 3 + (h % 2)
                    s_ = kv_ctr % 2
                    kv_ctr += 1
                    ksrc = KALL.ap().rearrange("(r f) t -> f r t", r=8)[h * 128:(h + 1) * 128, :, 0:(j + 1) * 128]
                    P.dma("sp", lambda e, s_=s_, ksrc=ksrc, j=j: e.dma_start(out=KT[s_][:, :, 0:(j + 1) * 128], in_=ksrc),
                          f"kt{s_}", r=("KALL",), w=(f"kt{s_}",))
                    vsrc = VALL.ap().rearrange("(r lb p) c -> p r lb c", r=8, lb=8)

                    def ldv(e, s_=s_, vsrc=vsrc, h=h, j=j):
                        return [e.dma_start(out=Vs[s_][:, r, 0:j + 1, 0:128], in_=vsrc[:, r, 0:j + 1, h * 128:(h + 1) * 128]) for r in range(8)]
                    P.dma("act", ldv, f"vs{s_}", r=("VALL",), w=(f"vs{s_}",), n=8)
                    blocks = [(kb_of(lb, r), lb, r) for lb in range(j + 1) for r in range(8)]
                    attn_blocks(
                        blocks, acc_b, (True, True),
                        lambda blk, s_=s_: KT[s_][:, blk[2], blk[1] * 128:(blk[1] + 1) * 128],
                        qj[jq][:, h, :],
                        lambda blk, n: MBT[:, blk[1] * 8 + blk[2], :],
                        lambda blk, s_=s_: Vs[s_][:, blk[2], blk[1], :],
                        lambda blk, h=h, j=j: sbias[:, (blk[0] - (8 * j + 8) + 64) * 8 + h:(blk[0] - (8 * j + 8) + 64) * 8 + h + 1],
                        (f"kt{s_}", f"vs{s_}", f"qj{jq}", "MBT"))
                    normalize_head(acc_b, oaj[jq][:, h * 128:(h + 1) * 128], f"oaj{jq}")
                for i in range(8):
                    P.op("pe", lambda e, i=i, jq=jq: e.transpose(PST[:, i * 128:(i + 1) * 128], oaj[jq][:, i * 128:(i + 1) * 128], ident[:]),
                         r=(f"oaj{jq}", "ident"), w=("pst",))
                P.op("act", lambda e, j=j: e.copy(out=oaT[:, :, j * 128:(j + 1) * 128], in_=PST[:, :].rearrange("p (a b) -> p a b", b=128)),
                     r=("pst",), w=("oaT",))
            P.wait_all("sp")
            P.run_block()
            if debug is not None and debug[0] == "p3a":
                dump_and_stop([oaT[:, i, :] for i in range(8)] + [Isc[:, 7, :, :].rearrange("p a b -> p (a b)"), theta[:], iw_sb[:].rearrange("p a b -> p (a b)")], stage=ctmp)
                return nc

        def layer_norm(rb, rkey, g_ap, b_ap, junk, s1, s2, mean, var, rstd, nb):
            P.op("act", lambda e: e.activation(out=junk, in_=rb, func=AF.Identity, accum_out=s1[:]), r=(rkey,), w=("lnjunk", "s1"))
            P.op("act", lambda e: e.activation(out=junk, in_=rb, func=AF.Square, accum_out=s2[:]), r=(rkey,), w=("lnjunk", "s2"))
            P.op("dve", lambda e: e.tensor_scalar(out=mean[:], in0=s1[:], scalar1=1.0 / D, scalar2=None, op0=ALU.mult), r=("s1",), w=("mean",))
            P.op("dve", lambda e: e.tensor_tensor(out=var[:], in0=mean[:], in1=mean[:], op=ALU.mult), r=("mean",), w=("var",))
            P.op("dve", lambda e: e.scalar_tensor_tensor(out=var[:], in0=s2[:], scalar=1.0 / D, in1=var[:], op0=ALU.mult, op1=ALU.subtract),
                 r=("s2", "var"), w=("var",))
            P.op("dve", lambda e: e.tensor_scalar(out=var[:], in0=var[:], scalar1=EPS, scalar2=None, op0=ALU.add), r=("var",), w=("var",))
            P.op("act", lambda e: e.sqrt(out=rstd[:], in_=var[:]), r=("var",), w=("rstd",))
            P.op("dve", lambda e: e.reciprocal(out=rstd[:], in_=rstd[:]), r=("rstd",), w=("rstd",))
            P.op("dve", lambda e: e.scalar_tensor_tensor(out=nb[:], in0=mean[:], scalar=-1.0, in1=rstd[:], op0=ALU.mult, op1=ALU.mult),
                 r=("mean", "rstd"), w=("nb",))
            P.op("act", lambda e: e.activation(out=rb, in_=rb, func=AF.Identity, bias=nb[:], scale=rstd[:]), r=(rkey, "nb", "rstd"), w=(rkey,))
            P.op("dve", lambda e: e.tensor_tensor(out=rb, in0=rb, in1=g_ap, op=ALU.mult), r=(rkey, "lnp"), w=(rkey,))
            P.op("dve", lambda e: e.tensor_tensor(out=rb, in0=rb, in1=b_ap, op=ALU.add), r=(rkey, "lnp"), w=(rkey,))

        with ExitStack() as outer:
            hT = outer.enter_context(sb("hT", [128, KC, T]))
            s1 = outer.enter_context(sb("s1", [128, 1], F32))
            s2 = outer.enter_context(sb("s2", [128, 1], F32))
            mean = outer.enter_context(sb("mean", [128, 1], F32))
            var = outer.enter_context(sb("var", [128, 1], F32))
            rstd = outer.enter_context(sb("rstd", [128, 1], F32))
            nb_ = outer.enter_context(sb("nb", [128, 1], F32))
            with ExitStack() as mid_scope:
                mT = mid_scope.enter_context(sb("mT", [128, KC, T]))
                with ExitStack() as ph:
                    xT = ph.enter_context(sb("xT2", [128, KC, T]))
                    WGA = [ph.enter_context(sb(f"wga{i}", [128, KC, 256])) for i in range(2)]
                    WGB = [ph.enter_context(sb(f"wgb{i}", [128, KC, 256])) for i in range(2)]
                    WAs = [ph.enter_context(sb(f"was{i}", [128, 8, 256])) for i in range(2)]
                    WBs = [ph.enter_context(sb(f"wbs{i}", [128, 4, 256])) for i in range(2)]
                    sga = [ph.enter_context(sb(f"sga{i}", [128, 512], F32)) for i in range(2)]
                    sgb = [ph.enter_context(sb(f"sgb{i}", [128, 512], F32)) for i in range(2)]
                    m1 = [ph.enter_context(sb(f"m1{i}", [128, 512], F32)) for i in range(2)]
                    m2 = [ph.enter_context(sb(f"m2{i}", [128, 512], F32)) for i in range(2)]

                    def ld_xT2(e):
                        src = xT_d.rearrange("(kc p) t -> p kc t", p=128)
                        return [e.dma_start(out=xT[:, 4 * i:4 * i + 4, :], in_=src[:, 4 * i:4 * i + 4, :]) for i in range(4)]
                    P.dma("pool", ld_xT2, "xT2", w=("xT",), n=4)
                    it = 0
                    for cc in range(8):
                        ws = cc % 2
                        c0 = cc * 256
                        for (tl, Wd_, key, col, nm) in ((WGA, WQ, "WQ", Q_GA + c0, "wga"), (WGB, WQ, "WQ", Q_GB + c0, "wgb"),
                                                       (WAs, WA, "WA", c0, "was"), (WBs, WB, "WB", c0, "wbs")):
                            src = Wd_.ap()[:, col:col + 256].rearrange("(kc p) n -> p kc n", p=128)
                            P.dma("sp", lambda e, t_=tl[ws], src=src: e.dma_start(out=t_[:], in_=src), f"{nm}{ws}", r=(key,), w=(f"{nm}{ws}",))
                        for fc in range(2):
                            for half in range(2):
                                tsl = slice(half * 512, (half + 1) * 512)
                                fsl = slice(fc * 128, (fc + 1) * 128)
                                bs = [nbank() for _ in range(4)]
                                for kc in range(8):
                                    P.op("pe", lambda e, b=bs[0], ws=ws, kc=kc, fsl=fsl, tsl=tsl: e.matmul(PS[b][:, :], lhsT=WAs[ws][:, kc, fsl], rhs=oaT[:, kc, tsl],
                                                                                                     start=(kc == 0), stop=(kc == 7)), r=(f"was{ws}", "oaT"), w=(f"ps{bs[0]}",))
                                for kc in range(4):
                                    P.op("pe", lambda e, b=bs[1], ws=ws, kc=kc, fsl=fsl, tsl=tsl: e.matmul(PS[b][:, :], lhsT=WBs[ws][:, kc, fsl], rhs=obT[:, kc, tsl],
                                                                                                     start=(kc == 0), stop=(kc == 3)), r=(f"wbs{ws}", "obT"), w=(f"ps{bs[1]}",))
                                for kc in range(KC):
                                    P.op("pe", lambda e, b=bs[2], ws=ws, kc=kc, fsl=fsl, tsl=tsl: e.matmul(PS[b][:, :], lhsT=WGA[ws][:, kc, fsl], rhs=xT[:, kc, tsl],
                                                                                                     start=(kc == 0), stop=(kc == KC - 1)), r=(f"wga{ws}", "xT"), w=(f"ps{bs[2]}",))
                                for kc in range(KC):
                                    P.op("pe", lambda e, b=bs[3], ws=ws, kc=kc, fsl=fsl, tsl=tsl: e.matmul(PS[b][:, :], lhsT=WGB[ws][:, kc, fsl], rhs=xT[:, kc, tsl],
                                                                                                     start=(kc == 0), stop=(kc == KC - 1)), r=(f"wgb{ws}", "xT"), w=(f"ps{bs[3]}",))
                                k = it % 2
                                it += 1
                                P.op("act", lambda e, k=k, b=bs[2]: e.activation(out=sga[k][:], in_=PS[b][:, :], func=AF.Sigmoid), r=(f"ps{bs[2]}",), w=(f"sga{k}",))
                                P.op("act", lambda e, k=k, b=bs[3]: e.activation(out=sgb[k][:], in_=PS[b][:, :], func=AF.Sigmoid), r=(f"ps{bs[3]}",), w=(f"sgb{k}",))
                                P.op("dve", lambda e, k=k, b=bs[0]: e.tensor_tensor(out=m1[k][:], in0=sga[k][:], in1=PS[b][:, :], op=ALU.mult), r=(f"sga{k}", f"ps{bs[0]}"), w=(f"m1{k}",))
                                P.op("dve", lambda e, k=k, b=bs[1]: e.tensor_tensor(out=m2[k][:], in0=sgb[k][:], in1=PS[b][:, :], op=ALU.mult), r=(f"sgb{k}", f"ps{bs[1]}"), w=(f"m2{k}",))
                                P.op("dve", lambda e, k=k, cc=cc, fc=fc, tsl=tsl: e.tensor_tensor(out=mT[:, cc * 2 + fc, tsl], in0=m1[k][:], in1=m2[k][:], op=ALU.add),
                                     r=(f"m1{k}", f"m2{k}"), w=("mT",))
                    P.wait_all("sp")
                    P.run_block()
                    if debug is not None and debug[0] == "p4a":
                        dump_and_stop([mT[:, i, :] for i in range(16)])
                        return nc
                with ExitStack() as ph:
                    WOs = [ph.enter_context(sb(f"wos{i}", [128, KC, 512])) for i in range(2)]
                    rbuf = [ph.enter_context(sb(f"rbuf{i}", [128, D], F32)) for i in range(4)]
                    lnp = ph.enter_context(sb("lnp1", [128, 2, D], F32))
                    junk = ph.enter_context(sb("lnjunk1", [128, D], F32))
                    hbf = [ph.enter_context(sb(f"hbf{i}", [128, D])) for i in range(2)]
                    P.dma("sp", lambda e: e.dma_start(out=lnp[:], in_=lnp_d[:, 0:2, :]), "lnp", w=("lnp",))
                    wo_ctr = 0
                    for lbg in range(2):
                        for i in range(4):
                            lb = lbg * 4 + i
                            P.dma("act", lambda e, i=i, lb=lb: e.dma_start(out=rbuf[i][:], in_=xtok_d[lb * 128:(lb + 1) * 128, :]), f"rbuf{i}", w=(f"rbuf{i}",))
                        for cc in range(4):
                            ws = wo_ctr % 2
                            wo_ctr += 1
                            src = WO.ap()[:, cc * 512:(cc + 1) * 512].rearrange("(kc p) n -> p kc n", p=128)
                            P.dma("sp", lambda e, ws=ws, src=src: e.dma_start(out=WOs[ws][:], in_=src), f"wos{ws}", r=("WO",), w=(f"wos{ws}",))
                            for i in range(4):
                                lb = lbg * 4 + i
                                b = nbank()
                                for kc in range(KC):
                                    P.op("pe", lambda e, b=b, ws=ws, kc=kc, lb=lb: e.matmul(PS[b][:, :], lhsT=mT[:, kc, lb * 128:(lb + 1) * 128], rhs=WOs[ws][:, kc, :],
                                                                                      start=(kc == 0), stop=(kc == KC - 1)), r=(f"wos{ws}", "mT"), w=(f"ps{b}",))
                                P.op("dve", lambda e, b=b, i=i, cc=cc: e.scalar_tensor_tensor(out=rbuf[i][:, cc * 512:(cc + 1) * 512], in0=rbuf[i][:, cc * 512:(cc + 1) * 512],
                                                                                            scalar=ALPHA, in1=PS[b][:, :], op0=ALU.mult, op1=ALU.add),
                                     r=(f"ps{b}", f"rbuf{i}"), w=(f"rbuf{i}",))
                        for i in range(4):
                            lb = lbg * 4 + i
                            layer_norm(rbuf[i][:], f"rbuf{i}", lnp[:, 0, :], lnp[:, 1, :], junk[:], s1, s2, mean, var, rstd, nb_)
                            P.dma("sp", lambda e, i=i, lb=lb: e.dma_start(out=hres.ap()[lb * 128:(lb + 1) * 128, :], in_=rbuf[i][:]), f"rbuf{i}_st", r=(f"rbuf{i}",), w=(f"hres{lb}",))
                            hb = lb % 2
                            P.op("act", lambda e, i=i, hb=hb: e.copy(out=hbf[hb][:], in_=rbuf[i][:]), r=(f"rbuf{i}",), w=(f"hbf{hb}",))
                            for q4 in range(2):
                                for kk in range(8):
                                    kc = q4 * 8 + kk
                                    P.op("pe", lambda e, kk=kk, kc=kc, hb=hb: e.transpose(PST[:, kk * 128:(kk + 1) * 128], hbf[hb][:, kc * 128:(kc + 1) * 128], ident[:]),
                                         r=(f"hbf{hb}", "ident"), w=("pst",))
                                evac(hT[:, q4 * 8:(q4 + 1) * 8, lb * 128:(lb + 1) * 128], PST[:, :].rearrange("p (a b) -> p a b", b=128), r=("pst",), w=("hT",))
                    P.wait_all("sp")
                    P.run_block()
                    if debug is not None and debug[0] == "p4b":
                        dump_and_stop([], dram_aps=[(dbg_d[:, :], hres.ap()[:, :])])
                        return nc
            with ExitStack() as ffn:
                aT = ffn.enter_context(sb("aT", [128, FC, T]))
                with ExitStack() as ph:
                    WGs = [ph.enter_context(sb(f"wgs{i}", [128, KC, 256])) for i in range(3)]
                    WUs = [ph.enter_context(sb(f"wus{i}", [128, KC, 256])) for i in range(3)]
                    sg = [ph.enter_context(sb(f"sg{i}", [128, 512], F32)) for i in range(2)]
                    it = 0
                    for cc in range(DFF // 256):
                        ws = cc % 3
                        for (tl, Wd_, key, nm) in ((WGs, WG, "WG", "wgs"), (WUs, WU, "WU", "wus")):
                            src = Wd_.ap()[:, cc * 256:(cc + 1) * 256].rearrange("(kc p) n -> p kc n", p=128)
                            P.dma("sp" if nm == "wgs" else "act", lambda e, t_=tl[ws], src=src: e.dma_start(out=t_[:], in_=src), f"{nm}{ws}", r=(key,), w=(f"{nm}{ws}",))
                        for fc in range(2):
                            fsl = slice(fc * 128, (fc + 1) * 128)
                            for half in range(2):
                                tsl = slice(half * 512, (half + 1) * 512)
                                bg, bu = nbank(), nbank()
                                for kc in range(KC):
                                    P.op("pe", lambda e, b=bg, ws=ws, kc=kc, fsl=fsl, tsl=tsl: e.matmul(PS[b][:, :], lhsT=WGs[ws][:, kc, fsl], rhs=hT[:, kc, tsl],
                                                                                                  start=(kc == 0), stop=(kc == KC - 1)), r=(f"wgs{ws}", "hT"), w=(f"ps{bg}",))
                                for kc in range(KC):
                                    P.op("pe", lambda e, b=bu, ws=ws, kc=kc, fsl=fsl, tsl=tsl: e.matmul(PS[b][:, :], lhsT=WUs[ws][:, kc, fsl], rhs=hT[:, kc, tsl],
                                                                                                  start=(kc == 0), stop=(kc == KC - 1)), r=(f"wus{ws}", "hT"), w=(f"ps{bu}",))
                                k = it % 2
                                it += 1
                                P.op("act", lambda e, k=k, b=bg: e.activation(out=sg[k][:], in_=PS[b][:, :], func=AF.Silu), r=(f"ps{bg}",), w=(f"sg{k}",))
                                P.op("dve", lambda e, k=k, b=bu, cc=cc, fc=fc, tsl=tsl: e.tensor_tensor(out=aT[:, cc * 2 + fc, tsl], in0=sg[k][:], in1=PS[b][:, :], op=ALU.mult),
                                     r=(f"sg{k}", f"ps{bu}"), w=("aT",))
                    P.wait_all("sp")
                    P.run_block()
                    if debug is not None and debug[0] == "p5":
                        dump_and_stop([aT[:, i, :] for i in (0, 1, 2, 3, 40, 41, 42, 43)] + [hT[:, i, :] for i in (0, 15)] + [sg[0][:], sg[1][:]])
                        return nc
                with ExitStack() as ph:
                    WDs = [oaT[:].rearrange("p a b -> p (a b)")[:, 0:5632].rearrange("p (a b) -> p a b", b=512),
                           ph.enter_context(sb("wds1", [128, 11, 512]))[:]]
                    lnp = ph.enter_context(sb("lnp2", [128, 2, D], F32))
                    junk = obT[:].rearrange("p a b -> p (a b)")[:, 0:D]
                    hbuf_t = [ph.enter_context(sb(f"hbuf{i}", [128, D], F32)) for i in range(2)]
                    hbuf = [t_[:] for t_ in hbuf_t]
                    P.dma("sp", lambda e: e.dma_start(out=lnp[:], in_=lnp_d[:, 2:4, :]), "lnp", w=("lnp",))
                    wd_ctr = 0
                    NG = 2
                    for lbg in range(NLB // NG):
                        for i in range(NG):
                            lb = lbg * NG + i
                            P.dma("act", lambda e, i=i, lb=lb: e.dma_start(out=hbuf[i], in_=hres.ap()[lb * 128:(lb + 1) * 128, :]), f"hbuf{i}", r=(f"hres{lb}",), w=(f"hbuf{i}",))
                        for cc in range(4):
                            bs = [nbank() for _ in range(NG)]
                            for kg in range(4):
                                ws = wd_ctr % 2
                                wd_ctr += 1
                                src = WD.ap()[kg * 1408:(kg + 1) * 1408, cc * 512:(cc + 1) * 512].rearrange("(kc p) n -> p kc n", p=128)
                                P.dma("sp", lambda e, ws=ws, src=src: e.dma_start(out=WDs[ws], in_=src), f"wds{ws}", r=("WD",), w=(f"wds{ws}",))
                                for kc in range(11):
                                    for i in range(NG):
                                        lb = lbg * NG + i
                                        P.op("pe", lambda e, b=bs[i], ws=ws, kc=kc, kg=kg, lb=lb: e.matmul(
                                            PS[b][:, :], lhsT=aT[:, kg * 11 + kc, lb * 128:(lb + 1) * 128], rhs=WDs[ws][:, kc, :],
                                            start=(kg == 0 and kc == 0), stop=(kg == 3 and kc == 10)), r=(f"wds{ws}", "aT"), w=(f"ps{bs[i]}",))
                            for i in range(NG):
                                P.op("dve", lambda e, b=bs[i], i=i, cc=cc: e.scalar_tensor_tensor(out=hbuf[i][:, cc * 512:(cc + 1) * 512], in0=hbuf[i][:, cc * 512:(cc + 1) * 512],
                                                                                                scalar=ALPHA, in1=PS[b][:, :], op0=ALU.mult, op1=ALU.add),
                                     r=(f"ps{bs[i]}", f"hbuf{i}"), w=(f"hbuf{i}",))
                        for i in range(NG):
                            lb = lbg * NG + i
                            layer_norm(hbuf[i], f"hbuf{i}", lnp[:, 0, :], lnp[:, 1, :], junk, s1, s2, mean, var, rstd, nb_)
                            P.dma("sp", lambda e, i=i, lb=lb: e.dma_start(out=out_d[lb * 128:(lb + 1) * 128, :], in_=hbuf[i]), f"hbuf{i}_st", r=(f"hbuf{i}",), w=(f"out{lb}",))
                    P.wait_all("sp")
                    P.run_block()
    return nc


def host_inputs(x, w_in, w_a, w_b, w_out, ln1_g, ln1_b, w_gate, w_up, w_down, ln2_g, ln2_b):
    x2 = np.asarray(x, dtype=np.float32)[0]
    w_in = np.asarray(w_in, dtype=np.float32)[0]
    cols = lambda a, n: np.arange(a, a + n)
    kvcols = np.concatenate([cols(C_AK, 1024), cols(C_BK, 1536), cols(C_IK, 64), cols(C_AV, 1024), cols(C_BV, 1536)])
    qcols = np.concatenate([cols(C_AQ, 1024), cols(C_IQ, 1024), cols(C_BQ, 1536), cols(C_IW, 16), cols(C_GA, 2048), cols(C_GB, 2048)])
    slopes = alibi_slopes()
    sl_d, sl_s = slopes[:12], slopes[12:]
    p = np.arange(128, dtype=np.float64)
    sbias = np.zeros((128, 64, 8))
    for mi in range(64):
        sbias[:, mi, :] = (128.0 * (mi - 64) + p)[:, None] * sl_s[None, :]
    dbias = np.zeros((128, 24, 12))
    for mi in range(24):
        dbias[:, mi, :] = (128.0 * (mi - 24) + p)[:, None] * sl_d[None, :]
    a1 = np.zeros((128, 12, 128), dtype=np.float32)
    for gh in range(12):
        sp_ = np.float32(sl_d[gh] / SCALE)
        hi = np.float32(sp_.astype(ml_dtypes.bfloat16))
        lo = np.float32(np.float32(sp_ - hi).astype(ml_dtypes.bfloat16))
        a1[0, gh, :], a1[1, gh, :], a1[2, gh, :], a1[3, gh, :] = hi, lo, hi, lo
    krel = np.zeros((128, 2, 1024), dtype=np.float32)
    for r in range(8):
        krel[:, 0, r * 128:(r + 1) * 128] = 128 * r + np.arange(128)
        krel[:, 1, r * 128:(r + 1) * 128] = 128 * (7 - r) + np.arange(128)
    lnp = np.stack([np.asarray(a, dtype=np.float32)[0] for a in (ln1_g, ln1_b, ln2_g, ln2_b)], 0)
    lnp = np.ascontiguousarray(np.broadcast_to(lnp[None], (128, 4, D)))
    shared = {
        "lnp": lnp, "ident": np.eye(128, dtype=np.float32),
        "sbias": sbias.reshape(128, -1).astype(np.float32), "dbias": dbias.reshape(128, -1).astype(np.float32),
        "a1": a1.reshape(128, -1), "krel": krel.reshape(128, -1),
    }
    wa, wb, wo = (np.asarray(a, dtype=np.float32)[0] for a in (w_a, w_b, w_out))
    wg, wu, wd = (np.asarray(a, dtype=np.float32)[0] for a in (w_gate, w_up, w_down))
    maps = []
    for c in range(NCORES):
        blks = blocks_of(c)
        tok = np.concatenate([np.arange(b * 128, (b + 1) * 128) for b in blks])
        xs = x2[tok]
        pos = tok.astype(np.float64).reshape(NLB, 128)
        posp = np.zeros((128, 16), dtype=np.float32)
        for lb in range(NLB):
            posp[:, lb] = pos[lb] - 1024.0 * lb
        posp[:, 8] = np.arange(128)
        posrow = np.ascontiguousarray(np.broadcast_to(pos.reshape(1, -1), (128, 1024))).astype(np.float32)
        b1 = np.zeros((128, 8, 128), dtype=np.float32)
        for lb in range(NLB):
            rel = pos[lb] - 1024.0 * (lb + 1)
            a = np.floor(rel / 128.0)
            bb = rel - 128.0 * a
            b1[0, lb], b1[1, lb], b1[2, lb], b1[3, lb] = -128.0 * a, -128.0 * a, -bb, -bb
        m = dict(shared)
        m.update({
            "xT": np.ascontiguousarray(xs.T), "xtok": np.ascontiguousarray(xs),
            "wkv": np.ascontiguousarray(w_in[256 * c:256 * (c + 1)][:, kvcols]),
            "wq": np.ascontiguousarray(w_in[256 * c:256 * (c + 1)][:, qcols]),
            "wa": np.ascontiguousarray(wa[128 * c:128 * (c + 1)]), "wb": np.ascontiguousarray(wb[64 * c:64 * (c + 1)]),
            "wo": np.ascontiguousarray(wo[256 * c:256 * (c + 1)]), "wg": np.ascontiguousarray(wg[256 * c:256 * (c + 1)]),
            "wu": np.ascontiguousarray(wu[256 * c:256 * (c + 1)]), "wd": np.ascontiguousarray(wd[704 * c:704 * (c + 1)]),
            "posp": posp, "posrow": posrow, "b1": b1.reshape(128, -1),
        })
        maps.append(m)
    return maps


def kernel(**inputs):
    maps = host_inputs(**inputs)
    nc = build_nc()
    res = run_bass_kernel_spmd(nc, maps, core_ids=list(range(NCORES)))
    out = np.zeros((1, SEQ, D), dtype=np.float32)
    for c in range(NCORES):
        y = np.asarray(res.results[c]["out"], dtype=np.float32)
        for lb, b in enumerate(blocks_of(c)):
            out[0, b * 128:(b + 1) * 128] = y[lb * 128:(lb + 1) * 128]
    return out
```
